# Optimizing a Trainium2 kernel written in Bass

```python
import math
import jax, jax.numpy as jnp
from jax import lax
import numpy as np

D_MODEL = 1024
BATCH = 4
SEQ = 8192
DEPTH = 2

N_HEADS = 16
HEAD_DIM = D_MODEL // N_HEADS
Q_BLOCK = 128
CONV_WIDTH = 3
D_FF = 2816
N_EXPERTS = 8
TOP_K = 2
RMS_EPS = 1e-6
N_EVEN = (DEPTH + 1) // 2
N_ODD = DEPTH // 2

kernel_name = "fox_shortconv_moe_hybrid"


def rmsnorm(x, g):
    x32 = x.astype(jnp.float32)
    y = x32 * lax.rsqrt(jnp.mean(x32 * x32, axis=-1, keepdims=True) + RMS_EPS)
    return (y * g.astype(jnp.float32)).astype(x.dtype)


def swiglu(t, w_gate, w_up, w_down):
    return (jax.nn.silu(t @ w_gate) * (t @ w_up)) @ w_down


def forgetting_attention(h, w_in, b_forget, w_out):
    bsz, seq, _ = h.shape
    proj = h @ w_in
    q = proj[..., :D_MODEL].reshape(bsz, seq, N_HEADS, HEAD_DIM)
    k = proj[..., D_MODEL:2 * D_MODEL].reshape(bsz, seq, N_HEADS, HEAD_DIM)
    v = proj[..., 2 * D_MODEL:3 * D_MODEL].reshape(bsz, seq, N_HEADS, HEAD_DIM)
    f_logit = proj[..., 3 * D_MODEL:] + b_forget
    log_f = jax.nn.log_sigmoid(f_logit.astype(jnp.float32))
    c = jnp.cumsum(log_f, axis=1).transpose(0, 2, 1)
    scale = 1.0 / math.sqrt(HEAD_DIM)
    key_pos = jnp.arange(seq)

    def query_block(i):
        start = i * Q_BLOCK
        qb = lax.dynamic_slice_in_dim(q, start, Q_BLOCK, axis=1)
        cb = lax.dynamic_slice_in_dim(c, start, Q_BLOCK, axis=2)
        s = jnp.einsum('bqhd,bkhd->bhqk', qb, k).astype(jnp.float32) * scale
        s = s + cb[:, :, :, None] - c[:, :, None, :]
        q_pos = start + jnp.arange(Q_BLOCK)
        causal = key_pos[None, :] <= q_pos[:, None]
        s = jnp.where(causal[None, None], s, -jnp.inf)
        p = jax.nn.softmax(s, axis=-1)
        return jnp.einsum('bhqk,bkhd->bqhd', p.astype(v.dtype), v)

    o = lax.map(query_block, jnp.arange(seq // Q_BLOCK))
    o = o.transpose(1, 0, 2, 3, 4).reshape(bsz, seq, D_MODEL)
    return o @ w_out


def short_conv_mixer(h, w_in, conv_w, w_out):
    proj = h @ w_in
    gate_b = proj[..., :D_MODEL]
    gate_c = proj[..., D_MODEL:2 * D_MODEL]
    xv = proj[..., 2 * D_MODEL:]
    u = gate_c * xv
    conv = lax.conv_general_dilated(
        u, conv_w[:, None, :].astype(u.dtype), window_strides=(1,),
        padding=[(CONV_WIDTH - 1, 0)],
        dimension_numbers=('NWC', 'WIO', 'NWC'),
        feature_group_count=D_MODEL)
    return (gate_b * conv) @ w_out


def expert_swiglu(h, w_router, w_gate, w_up, w_down):
    bsz, seq, _ = h.shape
    t = h.reshape(bsz * seq, D_MODEL)
    logits = (t @ w_router).astype(jnp.float32)
    top_v, top_i = lax.top_k(logits, TOP_K)
    top_w = jax.nn.softmax(top_v, axis=-1)
    gates = jnp.sum(jax.nn.one_hot(top_i, N_EXPERTS, dtype=jnp.float32) * top_w[..., None], axis=1)
    out = jnp.zeros_like(t)
    for e in range(N_EXPERTS):
        y = swiglu(t, w_gate[e], w_up[e], w_down[e])
        out = out + gates[:, e:e + 1].astype(t.dtype) * y
    return out.reshape(bsz, seq, D_MODEL)


def setup_inputs(seed: int = 0) -> dict:
    key = jax.random.key(seed)
    ks = jax.random.split(key, 20)
    D, H, F, E = D_MODEL, N_HEADS, D_FF, N_EXPERTS
    nrm = lambda k, shape, fan_in: jax.random.normal(k, shape, jnp.float32) * fan_in ** -0.5
    gain = lambda k, shape: 1.0 + 0.02 * jax.random.normal(k, shape, jnp.float32)
    return {
        "x": jax.random.normal(ks[0], (BATCH, SEQ, D), jnp.float32),
        "attn_norm": gain(ks[1], (N_EVEN, D)),
        "attn_w_in": nrm(ks[2], (N_EVEN, D, 3 * D + H), D),
        "attn_b_forget": jax.random.uniform(ks[3], (N_EVEN, H), jnp.float32, 1.0, 5.0),
        "attn_w_out": nrm(ks[4], (N_EVEN, D, D), D),
        "ffn_norm": gain(ks[5], (N_EVEN, D)),
        "ffn_w_gate": nrm(ks[6], (N_EVEN, D, F), D),
        "ffn_w_up": nrm(ks[7], (N_EVEN, D, F), D),
        "ffn_w_down": nrm(ks[8], (N_EVEN, F, D), F),
        "conv_norm": gain(ks[9], (N_ODD, D)),
        "conv_w_in": nrm(ks[10], (N_ODD, D, 3 * D), D),
        "conv_w": nrm(ks[11], (N_ODD, CONV_WIDTH, D), CONV_WIDTH),
        "conv_w_out": nrm(ks[12], (N_ODD, D, D), D),
        "moe_norm": gain(ks[13], (N_ODD, D)),
        "moe_w_router": nrm(ks[14], (N_ODD, D, E), D),
        "moe_w_gate": nrm(ks[15], (N_ODD, E, D, F), D),
        "moe_w_up": nrm(ks[16], (N_ODD, E, D, F), D),
        "moe_w_down": nrm(ks[17], (N_ODD, E, F, D), F),
        "final_norm": gain(ks[18], (D,)),
    }


def reference(x, attn_norm, attn_w_in, attn_b_forget, attn_w_out,
              ffn_norm, ffn_w_gate, ffn_w_up, ffn_w_down,
              conv_norm, conv_w_in, conv_w, conv_w_out,
              moe_norm, moe_w_router, moe_w_gate, moe_w_up, moe_w_down,
              final_norm):
    h = x
    for i in range(DEPTH):
        j = i // 2
        if i % 2 == 0:
            h = h + forgetting_attention(rmsnorm(h, attn_norm[j]), attn_w_in[j],
                                         attn_b_forget[j], attn_w_out[j])
            h = h + swiglu(rmsnorm(h, ffn_norm[j]), ffn_w_gate[j], ffn_w_up[j], ffn_w_down[j])
        else:
            h = h + short_conv_mixer(rmsnorm(h, conv_norm[j]), conv_w_in[j],
                                     conv_w[j], conv_w_out[j])
            h = h + expert_swiglu(rmsnorm(h, moe_norm[j]), moe_w_router[j],
                                  moe_w_gate[j], moe_w_up[j], moe_w_down[j])
    return rmsnorm(h, final_norm)
```

```python
import contextlib
import numpy as np
import concourse.bass as bass
import concourse.mybir as mybir
from concourse.bass_utils import run_bass_kernel_spmd

F32 = mybir.dt.float32
BF16 = mybir.dt.bfloat16
AF = mybir.ActivationFunctionType
ALU = mybir.AluOpType
AX = mybir.AxisListType

ENGS = ["pe", "act", "dve", "pool", "sp"]
D = 1024
F = 2816
NE = 8
NH = 16
NFT = F // 128
FP = 256
NPC = F // FP
EPS = 1e-6


class Buf:
    __slots__ = ("name", "w", "r", "dsem", "dcnt", "bg")

    def __init__(self, name, bg=False):
        self.name = name
        self.w = None
        self.r = []
        self.dsem = None
        self.dcnt = 0
        self.bg = bg


class Sched:
    def __init__(self):
        self.ops = {e: [] for e in ENGS}
        self.cnt = {e: 0 for e in ENGS}
        self.seen = {e: {} for e in ENGS}
        self.ndsem = 0
        self.dbufs = []

    def _deps(self, reads, writes):
        evs = []
        for b in reads:
            if b.w is not None:
                evs.append(b.w)
        for b in writes:
            if b.w is not None:
                evs.append(b.w)
            evs.extend(b.r)
        return evs

    def _commit(self, ev, reads, writes):
        for b in reads:
            b.r.append(ev)
        for b in writes:
            b.w = ev
            b.r = []

    def _waits(self, eng, evs):
        seen = self.seen[eng]
        out = {}
        for (k, v) in evs:
            if k == "pe" and eng == "pe":
                continue
            if seen.get(k, 0) >= v:
                continue
            if out.get(k, 0) < v:
                out[k] = v
        for k, v in out.items():
            seen[k] = v
        return list(out.items())

    def op(self, eng, fn, reads=(), writes=()):
        evs = self._deps(reads, writes)
        waits = self._waits(eng, evs)
        self.cnt[eng] += 1
        ev = (eng, self.cnt[eng])
        self.ops[eng].append((fn, waits, (eng, 1)))
        self._commit(ev, reads, writes)
        return ev

    def dma(self, eng, fn, reads=(), writes=(), disjoint=False):
        evs = self._deps(reads, () if disjoint else writes)
        waits = self._waits(eng, evs)
        b = writes[0]
        if b.dsem is None:
            b.dsem = ("d", self.ndsem)
            self.ndsem += 1
            self.dbufs.append(b)
        b.dcnt += 16
        ev = (b.dsem, b.dcnt)
        self.ops[eng].append((fn, waits, (b.dsem, 16)))
        self._commit(ev, reads, writes)
        return ev

    def wait_all(self, eng, bufs):
        evs = [b.w for b in bufs if b.w is not None]
        waits = self._waits(eng, evs)
        if waits:
            self.ops[eng].append((None, waits, None))

    def barrier(self):
        evs = [(e, self.cnt[e]) for e in ENGS if e != "sp" and self.cnt[e] > 0]
        evs += [(b.dsem, b.dcnt) for b in self.dbufs if not b.bg]
        for e in ENGS:
            waits = self._waits(e, evs)
            if waits:
                self.ops[e].append((None, waits, None))

    def emit(self, nc, stack):
        sems = {}
        for e in ENGS:
            sems[e] = stack.enter_context(nc.semaphore("s_" + e))
        for i in range(self.ndsem):
            sems[("d", i)] = stack.enter_context(nc.semaphore("d%d" % i))
        block = stack.enter_context(nc.Block())

        def replay(name):
            def run(engine):
                for (fn, waits, inc) in self.ops[name]:
                    for (k, v) in waits:
                        engine.wait_ge(sems[k], v)
                    if fn is None:
                        continue
                    ins = fn(engine)
                    if inc is not None:
                        ins.then_inc(sems[inc[0]], inc[1])
            return run

        block.tensor(replay("pe"))
        block.scalar(replay("act"))
        block.vector(replay("dve"))
        block.gpsimd(replay("pool"))
        block.sync(replay("sp"))


class Ring:
    def __init__(self, name, aps):
        self.aps = aps
        self.bufs = [Buf("%s%d" % (name, i)) for i in range(len(aps))]
        self.i = 0

    def next(self):
        k = self.i % len(self.aps)
        self.i += 1
        return self.aps[k], self.bufs[k]


class Arena:
    def __init__(self, t, size):
        self.t = t
        self.size = size
        self.off = 0

    def reset(self):
        self.off = 0

    def take(self, shape, dt=BF16):
        n = 1
        for s in shape[1:]:
            n *= s
        nw = n if dt == F32 else (n + 1) // 2
        assert self.off + nw <= self.size, ("arena overflow", self.off, nw, self.size)
        v = self.t[:, self.off:self.off + nw]
        self.off += nw
        if dt != F32:
            v = v.bitcast(dt)[:, 0:n]
        if len(shape) == 3:
            v = v.rearrange("p (a b) -> p a b", a=shape[1], b=shape[2])
        elif len(shape) == 4:
            v = v.rearrange("p (a b c) -> p a b c", a=shape[1], b=shape[2], c=shape[3])
        return v


def build(NBO, NBP, dbg=False, upto='E', nex=NE):
    T = NBO * 512
    TP = NBP * 512
    TK = TP + T
    TO = T + 128
    NKT = TK // 128
    NPT = TP // 128
    QPOS0 = TP - 128
    blocks = [(0, 128)] + [(128 + 512 * i, 512) for i in range(NBO)]

    nc = bass.Bass("TRN2", target_bir_lowering=False)
    inp = lambda name, shape: nc.dram_tensor(name, shape, F32, kind="ExternalInput").ap()
    skind = "ExternalOutput" if dbg else "Internal"
    scr = lambda name, shape, dt: nc.dram_tensor(name, shape, dt, kind=skind).ap()
    xo = inp("xo", [T, D])
    xp = inp("xp", [TP, D])
    flag_d = inp("flag", [128, 1])
    ident_d = inp("ident", [128, 128])
    mask_d = inp("masktri", [128, 128])
    attn_norm = inp("attn_norm", [1, D])
    attn_w_in = inp("attn_w_in", [D, 3 * D + NH])
    attn_b = inp("attn_b_forget", [NH, 1])
    attn_w_out = inp("attn_w_out", [D, D])
    ffn_norm = inp("ffn_norm", [1, D])
    ffn_w_gate = inp("ffn_w_gate", [D, F])
    ffn_w_up = inp("ffn_w_up", [D, F])
    ffn_w_down = inp("ffn_w_down", [F, D])
    conv_norm = inp("conv_norm", [1, D])
    conv_w_in = inp("conv_w_in", [D, 3 * D])
    conv_wT = inp("conv_wT", [D, 3])
    conv_w_out = inp("conv_w_out", [D, D])
    moe_norm = inp("moe_norm", [1, D])
    moe_w_router = inp("moe_w_router", [D, NE])
    moe_w_gate = inp("moe_w_gate", [nex, D, F])
    moe_w_up = inp("moe_w_up", [nex, D, F])
    moe_w_down = inp("moe_w_down", [nex, F, D])
    final_norm = inp("final_norm", [1, D])
    out_d = nc.dram_tensor("out", [T, D], F32, kind="ExternalOutput").ap()

    QT = scr("QT", [D, TO], BF16)
    CQ = scr("CQ", [NH, TO], BF16)
    KT = scr("KT", [D, TK], BF16)
    VS = scr("VS", [NH, 128, NKT, 64], BF16)
    OT = scr("OT", [D, TO], BF16)
    HS = scr("HS", [TO, D], F32)
    scr2 = lambda name, shape, dt: nc.dram_tensor(name, shape, dt, kind="Internal").ap()
    G0 = scr2("G0", [D, F], BF16)
    U0 = scr2("U0", [D, F], BF16)
    D0 = scr2("D0", [F, D], BF16)
    GM = scr2("GM", [nex, D, F], BF16)
    UM = scr2("UM", [nex, D, F], BF16)
    DM = scr2("DM", [nex, F, D], BF16)
    bQT, bCQ, bKT, bVS, bOT, bHS = [Buf(n) for n in ["QT", "CQ", "KT", "VS", "OT", "HS"]]
    bOUT = Buf("OUT")

    S = Sched()
    st = contextlib.ExitStack()
    NAR = 43 * 1024
    NCF = 256 + NKT * NH + 16
    ar_t = st.enter_context(nc.sbuf_tensor("arena", [128, NAR], F32))
    cst_bf = st.enter_context(nc.sbuf_tensor("cstbf", [128, 256], BF16))
    cst_f = st.enter_context(nc.sbuf_tensor("cstf", [128, NCF], F32))
    PB = [st.enter_context(nc.psum_tensor("pb%d" % i, [128, 512], F32)) for i in range(7)]
    PT = st.enter_context(nc.psum_tensor("ptb", [128, 8, 128], BF16))
    PBb = [Buf("pb%d" % i) for i in range(7)]
    PTb = Buf("ptb")
    ar = Arena(ar_t, NAR)

    identb = cst_bf[:, 0:128]
    maskb = cst_bf[:, 128:256]
    identf = cst_f[:, 0:128]
    tmpf = cst_f[:, 128:256]
    cneg = cst_f[:, 256:256 + NKT * NH].rearrange("p (j h) -> p j h", j=NKT, h=NH)
    c0_ = 256 + NKT * NH
    flag = cst_f[:, c0_:c0_ + 1]
    nbf = cst_f[0:NH, c0_ + 1:c0_ + 2]
    ss_r = Ring("ss", [cst_f[:, c0_ + 2 + i:c0_ + 3 + i] for i in range(4)])
    b_const, b_cneg, b_tmpf = Buf("const"), Buf("cneg"), Buf("tmpf")

    S.dma("sp", lambda e: e.dma_start(out=identf, in_=ident_d[:, :]), writes=[b_const])
    S.dma("sp", lambda e: e.dma_start(out=tmpf, in_=mask_d[:, :]), writes=[b_tmpf])
    S.dma("sp", lambda e: e.dma_start(out=flag, in_=flag_d[:, :]), writes=[b_const])
    S.dma("sp", lambda e: e.dma_start(out=nbf, in_=attn_b[:, :]), writes=[b_const])
    S.op("dve", lambda e: e.tensor_copy(out=identb, in_=identf), reads=[b_const], writes=[b_const])
    S.op("dve", lambda e: e.tensor_copy(out=maskb, in_=tmpf), reads=[b_tmpf], writes=[b_const])
    S.op("dve", lambda e: e.tensor_scalar(out=nbf, in0=nbf, scalar1=-1.0, scalar2=None, op0=ALU.mult),
         reads=[b_const], writes=[b_const])

    tog = [0]

    def evac(out_ap, in_ap, reads, writes, scale=None, eng=None):
        if eng is None:
            tog[0] ^= 1
            eng = "act" if tog[0] else "dve"
        if eng == "act":
            if scale is None:
                return S.op("act", lambda e: e.copy(out=out_ap, in_=in_ap), reads=reads, writes=writes)
            return S.op("act", lambda e: e.activation(out=out_ap, in_=in_ap, func=AF.Copy, scale=scale), reads=reads, writes=writes)
        if scale is None:
            return S.op("dve", lambda e: e.tensor_copy(out=out_ap, in_=in_ap), reads=reads, writes=writes)
        return S.op("dve", lambda e: e.tensor_scalar(out=out_ap, in0=in_ap, scalar1=scale, scalar2=None, op0=ALU.mult),
                    reads=reads, writes=writes)

    pb_i = [0]

    def next_pb(lo=0, hi=7):
        k = lo + pb_i[0] % (hi - lo)
        pb_i[0] += 1
        return PB[k], PBb[k]

    def mm_group(out_ap, b_out, lhs_fn, rhs_fn, n, reads):
        def f(e):
            for c in range(n):
                ins = e.matmul(out_ap, lhsT=lhs_fn(c), rhs=rhs_fn(c), start=(c == 0), stop=(c == n - 1))
            return ins
        return S.op("pe", f, reads=reads, writes=[b_out])

    def norm_tile(src, b_src, g, b_g, hn, b_hn, sq, b_sq, out_f32=None, b_of=None):
        ss, b_ss = ss_r.next()
        S.op("act", lambda e: e.activation(out=sq, in_=src, func=AF.Square, accum_out=ss), reads=[b_src], writes=[b_sq, b_ss])
        S.op("dve", lambda e: e.tensor_scalar(out=ss, in0=ss, scalar1=1.0 / D, scalar2=EPS, op0=ALU.mult, op1=ALU.add),
             reads=[b_ss], writes=[b_ss])
        S.op("act", lambda e: e.activation(out=ss, in_=ss, func=AF.Sqrt), reads=[b_ss], writes=[b_ss])
        S.op("dve", lambda e: e.reciprocal(out=ss, in_=ss), reads=[b_ss], writes=[b_ss])
        dst, b_dst = (hn, b_hn) if out_f32 is None else (out_f32, b_of)
        S.op("dve", lambda e: e.scalar_tensor_tensor(out=dst, in0=src, scalar=ss, in1=g, op0=ALU.mult, op1=ALU.mult),
             reads=[b_src, b_ss, b_g], writes=[b_dst])

    def transpose_to(hn, b_hn, dst, b_dst):
        def tr(e):
            for c in range(8):
                ins = e.transpose(out=PT[:, c, :], in_=hn[:, c * 128:(c + 1) * 128], identity=identb)
            return ins
        S.op("pe", tr, reads=[b_hn, b_const], writes=[PTb])
        evac(dst, PT[:, :, :], [PTb], [b_dst])

    def ext_tile_src(i):
        if i == 0:
            return xp[TP - 128:TP, :]
        return xo[(i - 1) * 128:i * 128, :]

    def load_g(dst, src, b):
        S.dma("sp", lambda e: e.dma_start(out=dst, in_=src[0, :].partition_broadcast(128)), writes=[b])

    def cast2d(dst, src, buf, rows, nsplit=2):
        step = rows // nsplit
        for i in range(nsplit):
            S.dma("pool", lambda e, i=i: e.dma_start(out=dst[i * step:(i + 1) * step, :], in_=src[i * step:(i + 1) * step, :]),
                  writes=[buf])

    def ffn(hnT, b_hnT, w, ntile, Gd, Ud, Dd, bG, bU, bDn, gu_r, dn_r, actT, b_actT, tmp_r, consume):
        for pc in range(NPC):
            gup, b_gup = gu_r.next()
            S.dma("sp", lambda e, gup=gup, pc=pc: e.dma_start(out=gup[:, 0, :, :], in_=Gd[:, pc * FP:(pc + 1) * FP].rearrange("(c p) n -> p c n", p=128)),
                  reads=[bG], writes=[b_gup])
            S.dma("sp", lambda e, gup=gup, pc=pc: e.dma_start(out=gup[:, 1, :, :], in_=Ud[:, pc * FP:(pc + 1) * FP].rearrange("(c p) n -> p c n", p=128)),
                  reads=[bU], writes=[b_gup])
            for s in range(FP // 128):
                ft = pc * (FP // 128) + s
                pg, b_pg = next_pb(0, 4)
                pu, b_pu = next_pb(0, 4)
                mm_group(pg[:, 0:w], b_pg, lambda c, gup=gup, s=s: gup[:, 0, c, s * 128:(s + 1) * 128], lambda c: hnT[:, c, 0:w], 8, [b_gup, b_hnT])
                mm_group(pu[:, 0:w], b_pu, lambda c, gup=gup, s=s: gup[:, 1, c, s * 128:(s + 1) * 128], lambda c: hnT[:, c, 0:w], 8, [b_gup, b_hnT])
                tm, b_tm = tmp_r.next()
                S.op("act", lambda e, tm=tm, pg=pg: e.activation(out=tm[:, 0:w], in_=pg[:, 0:w], func=AF.Silu), reads=[b_pg], writes=[b_tm])
                S.op("dve", lambda e, tm=tm, pu=pu, ft=ft: e.tensor_tensor(out=actT[:, ft, 0:w], in0=pu[:, 0:w], in1=tm[:, 0:w], op=ALU.mult),
                     reads=[b_pu, b_tm], writes=[b_actT])
        for half in range(2):
            dh, b_dh = dn_r.next()
            S.dma("sp", lambda e, dh=dh, half=half: e.dma_start(out=dh, in_=Dd[:, half * 512:(half + 1) * 512].rearrange("(c p) n -> p c n", p=128)),
                  reads=[bDn], writes=[b_dh])
            for t in range(ntile):
                pb, b_pb = next_pb(4, 7)
                mm_group(pb[:, :], b_pb, lambda c, t=t: actT[:, c, t * 128:(t + 1) * 128], lambda c, dh=dh: dh[:, c, :], NFT, [b_actT, b_dh])
                consume(t, half, pb, b_pb)

    bW = {n: Buf(n, bg=True) for n in ["G0", "U0", "D0"]}
    for e_ in range(nex):
        for n in "GUD":
            bW["%s%d" % (n, e_)] = Buf("%s%d" % (n, e_), bg=True)

    ar.reset()
    Wi = ar.take([128, 8, 3 * D + NH])
    hn_r = Ring("hn", [ar.take([128, D]) for _ in range(2)])
    hnT_r = Ring("hnT", [ar.take([128, 8, 512]) for _ in range(2)])
    stg_r = Ring("stg", [ar.take([128, 512]) for _ in range(3)])
    vst_r = Ring("vst", [ar.take([128, D]) for _ in range(2)])
    cqb = ar.take([128, TO])
    sq = ar.take([128, D]); b_sq = Buf("sq")
    x_r = Ring("x", [ar.take([128, D], F32) for _ in range(2)])
    gA = ar.take([128, D], F32); b_gA = Buf("gA")
    nlogf = ar.take([128, TK], F32); b_nlogf = Buf("nlogf")
    ncum = ar.take([128, TK], F32); b_ncum = Buf("ncum")
    b_win, b_cqb = Buf("win"), Buf("cqb")

    S.dma("pool", lambda e: e.dma_start(out=Wi, in_=attn_w_in.rearrange("(c p) n -> p c n", p=128)), writes=[b_win])
    load_g(gA, attn_norm, b_gA)
    cast2d(G0, ffn_w_gate, bW["G0"], D)
    cast2d(U0, ffn_w_up, bW["U0"], D)
    cast2d(D0, ffn_w_down, bW["D0"], F)
    for e_ in range(nex):
        cast2d(GM[e_], moe_w_gate[e_], bW["G%d" % e_], D)
        cast2d(UM[e_], moe_w_up[e_], bW["U%d" % e_], D)
        cast2d(DM[e_], moe_w_down[e_], bW["D%d" % e_], F)

    def phaseA_block(kb):
        hnT, b_hnT = hnT_r.next()
        for t in range(4):
            kt = kb * 4 + t
            xt, b_xt = x_r.next()
            src = xp[kt * 128:(kt + 1) * 128, :] if kt < NPT else xo[(kt - NPT) * 128:(kt - NPT + 1) * 128, :]
            S.dma("sp", lambda e, xt=xt, src=src: e.dma_start(out=xt, in_=src), writes=[b_xt])
            hn, b_hn = hn_r.next()
            norm_tile(xt, b_xt, gA, b_gA, hn, b_hn, sq, b_sq)
            transpose_to(hn, b_hn, hnT[:, :, t * 128:(t + 1) * 128], b_hnT)
        for ft in range(8):
            pb, b_pb = next_pb()
            mm_group(pb[:, :], b_pb, lambda c, ft=ft: Wi[:, c, D + ft * 128:D + (ft + 1) * 128], lambda c, hnT=hnT: hnT[:, c, :], 8, [b_win, b_hnT])
            sg, b_sg = stg_r.next()
            evac(sg, pb[:, :], [b_pb], [b_sg])
            S.dma("sp", lambda e, sg=sg, ft=ft, kb=kb: e.dma_start(out=KT[ft * 128:(ft + 1) * 128, kb * 512:(kb + 1) * 512], in_=sg),
                  reads=[b_sg], writes=[bKT], disjoint=True)
        if kb >= NBP - 1:
            for ft in range(8):
                pb, b_pb = next_pb()
                mm_group(pb[:, :], b_pb, lambda c, ft=ft: Wi[:, c, ft * 128:(ft + 1) * 128], lambda c, hnT=hnT: hnT[:, c, :], 8, [b_win, b_hnT])
                sg, b_sg = stg_r.next()
                evac(sg, pb[:, :], [b_pb], [b_sg], scale=0.125)
                if kb == NBP - 1:
                    S.dma("sp", lambda e, sg=sg, ft=ft: e.dma_start(out=QT[ft * 128:(ft + 1) * 128, 0:128], in_=sg[:, 384:512]),
                          reads=[b_sg], writes=[bQT], disjoint=True)
                else:
                    o0 = 128 + (kb - NBP) * 512
                    S.dma("sp", lambda e, sg=sg, ft=ft, o0=o0: e.dma_start(out=QT[ft * 128:(ft + 1) * 128, o0:o0 + 512], in_=sg),
                          reads=[b_sg], writes=[bQT], disjoint=True)
        for t in range(4):
            kt = kb * 4 + t
            vs_, b_vs = vst_r.next()
            for half in range(2):
                pb, b_pb = next_pb()
                mm_group(pb[:, :], b_pb, lambda c, hnT=hnT, t=t: hnT[:, c, t * 128:(t + 1) * 128],
                         lambda c, half=half: Wi[:, c, 2 * D + half * 512:2 * D + (half + 1) * 512], 8, [b_win, b_hnT])
                evac(vs_[:, half * 512:(half + 1) * 512], pb[:, :], [b_pb], [b_vs])
            S.dma("sp", lambda e, vs_=vs_, kt=kt: e.dma_start(out=VS.rearrange("h p j d -> p h j d")[:, :, kt, :],
                                                             in_=vs_.rearrange("p (h d) -> p h d", h=NH)),
                  reads=[b_vs], writes=[bVS], disjoint=True)
        pb, b_pb = next_pb()
        mm_group(pb[0:NH, :], b_pb, lambda c: Wi[:, c, 3 * D:3 * D + NH], lambda c, hnT=hnT: hnT[:, c, :], 8, [b_win, b_hnT])
        dst = nlogf[0:NH, kb * 512:(kb + 1) * 512]
        S.op("act", lambda e, pb=pb, dst=dst: e.activation(out=dst, in_=pb[0:NH, :], func=AF.Exp, bias=nbf, scale=-1.0),
             reads=[b_pb, b_const], writes=[b_nlogf])
        S.op("act", lambda e, dst=dst: e.activation(out=dst, in_=dst, func=AF.Ln, bias=1.0, scale=1.0), reads=[b_nlogf], writes=[b_nlogf])

    for kb in range(NBP + NBO):
        phaseA_block(kb)

    S.op("dve", lambda e: e.tensor_scalar(out=nlogf[0:NH, 0:TP], in0=nlogf[0:NH, 0:TP], scalar1=flag[0:NH, 0:1], scalar2=None, op0=ALU.mult),
         reads=[b_nlogf, b_const], writes=[b_nlogf])
    S.op("dve", lambda e: e.memset(ncum[0:NH, :], 1.0), writes=[b_ncum])
    S.op("dve", lambda e: e.tensor_tensor_scan(out=ncum[0:NH, :], data0=ncum[0:NH, :], data1=nlogf[0:NH, :], initial=0.0, op0=ALU.mult, op1=ALU.add),
         reads=[b_nlogf, b_ncum], writes=[b_ncum])
    S.op("dve", lambda e: e.tensor_scalar(out=cqb[0:NH, :], in0=ncum[0:NH, QPOS0:QPOS0 + TO], scalar1=-1.0, scalar2=None, op0=ALU.mult),
         reads=[b_ncum], writes=[b_cqb])
    S.dma("sp", lambda e: e.dma_start(out=CQ[:, :], in_=cqb[0:NH, :]), reads=[b_cqb], writes=[bCQ])
    for j0 in range(0, NKT, 32):
        nj = min(32, NKT - j0)
        pb, b_pb = next_pb()

        def trc(e, pb=pb, j0=j0, nj=nj):
            for j in range(nj):
                ins = e.transpose(out=pb[:, j * NH:(j + 1) * NH], in_=ncum[0:NH, (j0 + j) * 128:(j0 + j + 1) * 128], identity=identf[0:NH, 0:NH])
            return ins
        S.op("pe", trc, reads=[b_ncum, b_const], writes=[b_pb])
        S.op("dve", lambda e, pb=pb, j0=j0, nj=nj: e.tensor_copy(out=cneg[:, j0:j0 + nj, :], in_=pb[:, 0:nj * NH].rearrange("p (j h) -> p j h", j=nj, h=NH)),
             reads=[b_pb], writes=[b_cneg])
    S.barrier()
    if upto == 'A':
        S.emit(nc, st)
        st.close()
        return nc

    ar.reset()
    kta = [ar.take([128, TK]) for _ in range(2)]
    va = [ar.take([128, NKT, 128]) for _ in range(2)]
    qta = [ar.take([128, TO]) for _ in range(2)]
    P_r = Ring("P", [ar.take([128, 512]) for _ in range(4)])
    ost_r = Ring("ost", [ar.take([128, 512]) for _ in range(2)])
    rec_r = Ring("rec", [ar.take([128, 512], F32) for _ in range(2)])
    b_kta = [Buf("kta0"), Buf("kta1")]
    b_va = [Buf("va0"), Buf("va1")]
    b_qta = [Buf("qta0"), Buf("qta1")]
    for i in range(2):
        S.op("pool", lambda e, i=i: e.memset(kta[i][64:65, :], 1.0), writes=[b_kta[i]])
        S.op("pool", lambda e, i=i: e.memset(va[i][:, :, 64:128], 1.0), writes=[b_va[i]])
        S.op("dve", lambda e, i=i: e.tensor_scalar(out=va[i][:, 0:NPT, 64:128], in0=va[i][:, 0:NPT, 64:128], scalar1=flag[:, 0:1], scalar2=None, op0=ALU.mult),
             reads=[b_const], writes=[b_va[i]])
    S_banks = [0, 1, 2]
    O_banks = [3, 4]
    o_i = [0]
    s_i = [0]

    def head_loads(h):
        s = h % 2
        S.dma("sp", lambda e: e.dma_start(out=kta[s][0:64, :], in_=KT[64 * h:64 * h + 64, :]), reads=[bKT], writes=[b_kta[s]])
        for q4 in range(4):
            j0, j1 = q4 * NKT // 4, (q4 + 1) * NKT // 4
            S.dma("sp", lambda e, j0=j0, j1=j1: e.dma_start(out=va[s][:, j0:j1, 0:64], in_=VS[h, :, j0:j1, :]), reads=[bVS], writes=[b_va[s]])
        S.dma("sp", lambda e: e.dma_start(out=qta[s][0:64, :], in_=QT[64 * h:64 * h + 64, :]), reads=[bQT], writes=[b_qta[s]])
        S.dma("sp", lambda e: e.dma_start(out=qta[s][64:65, :], in_=CQ[h:h + 1, :]), reads=[bCQ], writes=[b_qta[s]])

    def attn_block(h, s, qoff, w):
            qs = QPOS0 + qoff
            jd = qs // 128
            jlast = (qs + w - 1) // 128
            ko = O_banks[o_i[0] % 2]; o_i[0] += 1
            po, b_po = PB[ko], PBb[ko]
            units = []
            for j in range(jlast + 1):
                jj = j - jd
                units.append((j, 128 * jj if jj > 0 else 0, jj >= 0))

            def emit_S(u):
                j, c0, diag = u
                kb_ = S_banks[s_i[0] % 3]; s_i[0] += 1
                ps, b_ps = PB[kb_], PBb[kb_]

                def f(e):
                    ins = e.matmul(ps[:, c0:w], lhsT=kta[s][0:65, j * 128:(j + 1) * 128], rhs=qta[s][0:65, qoff + c0:qoff + w],
                                   start=True, stop=(not diag))
                    if diag:
                        ins = e.matmul(ps[:, c0:c0 + 128], lhsT=identb, rhs=maskb, start=False, stop=True)
                    return ins
                S.op("pe", f, reads=[b_kta[s], b_qta[s], b_const], writes=[b_ps])
                pt, b_pt = P_r.next()
                S.op("act", lambda e: e.activation(out=pt[:, c0:w], in_=ps[:, c0:w], func=AF.Exp, bias=cneg[:, j, h:h + 1], scale=1.0),
                     reads=[b_ps, b_cneg], writes=[b_pt])
                return (pt, b_pt)

            def emit_PV(u, p):
                j, c0, diag = u
                pt, b_pt = p
                S.op("pe", lambda e: e.matmul(po[:, c0:w], lhsT=va[s][:, j, :], rhs=pt[:, c0:w], start=(j == 0), stop=(j == jlast)),
                     reads=[b_va[s], b_pt], writes=[b_po])

            pend = []
            LOOK = 2
            for idx, u in enumerate(units):
                pend.append((u, emit_S(u)))
                if len(pend) > LOOK:
                    uu, pp = pend.pop(0)
                    emit_PV(uu, pp)
            for uu, pp in pend:
                emit_PV(uu, pp)
            rc, b_rc = rec_r.next()
            S.op("dve", lambda e: e.tensor_scalar(out=rc[0:64, 0:w], in0=po[64:128, 0:w], scalar1=1e-30, scalar2=None, op0=ALU.add),
                 reads=[b_po], writes=[b_rc])
            S.op("dve", lambda e: e.reciprocal(out=rc[0:64, 0:w], in_=rc[0:64, 0:w]), reads=[b_rc], writes=[b_rc])
            og, b_og = ost_r.next()
            S.op("dve", lambda e: e.tensor_tensor(out=og[0:64, 0:w], in0=po[0:64, 0:w], in1=rc[0:64, 0:w], op=ALU.mult),
                 reads=[b_po, b_rc], writes=[b_og])
            S.dma("sp", lambda e: e.dma_start(out=OT[64 * h:64 * h + 64, qoff:qoff + w], in_=og[0:64, 0:w]),
                  reads=[b_og], writes=[bOT], disjoint=True)

    head_loads(0)
    for h in range(NH):
        s = h % 2
        if h + 1 < NH:
            head_loads(h + 1)
        for (qoff, w) in blocks:
            attn_block(h, s, qoff, w)
    S.barrier()
    if upto == 'B':
        S.emit(nc, st)
        st.close()
        return nc

    ar.reset()
    Wo = ar.take([128, 8, D]); b_wo = Buf("wo")
    otb_r = Ring("otb", [ar.take([128, 8, 512]) for _ in range(2)])
    hn_r = Ring("hn", [ar.take([128, D]) for _ in range(2)])
    hnT_r = Ring("hnT", [ar.take([128, 8, 512]) for _ in range(1)])
    gu_r = Ring("gu", [ar.take([128, 2, 8, FP]) for _ in range(3)])
    actT = ar.take([128, NFT, 512]); b_actT = Buf("actT")
    dn_r = Ring("dn", [ar.take([128, NFT, 512]) for _ in range(2)])
    tmp_r = Ring("tmp", [ar.take([128, 512]) for _ in range(2)])
    sq = ar.take([128, D]); b_sq = Buf("sq")
    x_r = Ring("x", [ar.take([128, D], F32) for _ in range(2)])
    h1 = [ar.take([128, D], F32) for _ in range(4)]
    b_h1 = [Buf("h1_%d" % i) for i in range(4)]
    gC = ar.take([128, D], F32); b_gC = Buf("gC")
    S.dma("pool", lambda e: e.dma_start(out=Wo, in_=attn_w_out.rearrange("(c p) n -> p c n", p=128)), writes=[b_wo])
    load_g(gC, ffn_norm, b_gC)
    def phaseC_block(qoff, w):
        ntile = w // 128
        otb, b_otb = otb_r.next()
        S.dma("sp", lambda e, otb=otb, qoff=qoff, w=w: e.dma_start(out=otb[:, :, 0:w], in_=OT[:, qoff:qoff + w].rearrange("(c p) t -> p c t", p=128)),
              reads=[bOT], writes=[b_otb])
        hnT, b_hnT = hnT_r.next()
        for t in range(ntile):
            it = qoff // 128 + t
            xt, b_xt = x_r.next()
            S.dma("sp", lambda e, xt=xt, it=it: e.dma_start(out=xt, in_=ext_tile_src(it)), writes=[b_xt])
            for half in range(2):
                pb, b_pb = next_pb(4, 7)
                mm_group(pb[:, :], b_pb, lambda c, otb=otb, t=t: otb[:, c, t * 128:(t + 1) * 128],
                         lambda c, half=half: Wo[:, c, half * 512:(half + 1) * 512], 8, [b_otb, b_wo])
                S.op("dve", lambda e, t=t, half=half, pb=pb, xt=xt: e.tensor_tensor(out=h1[t][:, half * 512:(half + 1) * 512], in0=pb[:, :],
                                                                                   in1=xt[:, half * 512:(half + 1) * 512], op=ALU.add),
                     reads=[b_pb, b_xt], writes=[b_h1[t]])
            hn, b_hn = hn_r.next()
            norm_tile(h1[t], b_h1[t], gC, b_gC, hn, b_hn, sq, b_sq)
            transpose_to(hn, b_hn, hnT[:, :, t * 128:(t + 1) * 128], b_hnT)

        def consume(t, half, pb, b_pb):
            S.op("dve", lambda e: e.tensor_tensor(out=h1[t][:, half * 512:(half + 1) * 512], in0=pb[:, :],
                                                  in1=h1[t][:, half * 512:(half + 1) * 512], op=ALU.add),
                 reads=[b_pb, b_h1[t]], writes=[b_h1[t]])
            if half == 1:
                it = qoff // 128 + t
                S.dma("sp", lambda e: e.dma_start(out=HS[it * 128:(it + 1) * 128, :], in_=h1[t]), reads=[b_h1[t]], writes=[bHS], disjoint=True)
        ffn(hnT, b_hnT, w, ntile, G0, U0, D0, bW["G0"], bW["U0"], bW["D0"], gu_r, dn_r, actT, b_actT, tmp_r, consume)

    for (qoff, w) in blocks:
        phaseC_block(qoff, w)
    S.barrier()
    if upto == 'C':
        S.emit(nc, st)
        st.close()
        return nc

    ar.reset()
    Wci = ar.take([128, 8, 3 * D]); b_wci = Buf("wci")
    Wco = ar.take([128, 8, D]); b_wco = Buf("wco")
    hn_r = Ring("hn", [ar.take([128, D]) for _ in range(2)])
    hnT_r = Ring("hnT", [ar.take([128, 8, 512]) for _ in range(1)])
    zT = ar.take([128, 8, 512]); b_zT = Buf("zT")
    sq = ar.take([128, D]); b_sq = Buf("sq")
    h2 = [ar.take([128, D], F32) for _ in range(4)]
    b_h2 = [Buf("h2_%d" % i) for i in range(4)]
    gD = ar.take([128, D], F32); b_gD = Buf("gD")
    uT = ar.take([128, 8, 2 + 512], F32); b_uT = [Buf("uT%d" % i) for i in range(8)]
    csb_r = Ring("csb", [ar.take([128, 512], F32) for _ in range(2)])
    y_r = Ring("y", [ar.take([128, 512], F32) for _ in range(2)])
    bsb_r = Ring("bsb", [ar.take([128, 512], F32) for _ in range(2)])
    cw = ar.take([128, 8, 3], F32); b_cw = Buf("cw")
    S.dma("pool", lambda e: e.dma_start(out=Wci, in_=conv_w_in.rearrange("(c p) n -> p c n", p=128)), writes=[b_wci])
    S.dma("pool", lambda e: e.dma_start(out=Wco, in_=conv_w_out.rearrange("(c p) n -> p c n", p=128)), writes=[b_wco])
    S.dma("sp", lambda e: e.dma_start(out=cw, in_=conv_wT.rearrange("(c p) k -> p c k", p=128)), writes=[b_cw])
    load_g(gD, conv_norm, b_gD)
    bHS2 = Buf("HS2")

    def phaseD_block(bi, qoff, w):
        ntile = w // 128
        hnT, b_hnT = hnT_r.next()
        for t in range(ntile):
            it = qoff // 128 + t
            S.dma("sp", lambda e, t=t, it=it: e.dma_start(out=h2[t], in_=HS[it * 128:(it + 1) * 128, :]), reads=[bHS], writes=[b_h2[t]])
            hn, b_hn = hn_r.next()
            norm_tile(h2[t], b_h2[t], gD, b_gD, hn, b_hn, sq, b_sq)
            transpose_to(hn, b_hn, hnT[:, :, t * 128:(t + 1) * 128], b_hnT)
        for dt_ in range(8):
            pc_, b_pc = next_pb(0, 4)
            mm_group(pc_[:, 0:w], b_pc, lambda c, dt_=dt_: Wci[:, c, D + dt_ * 128:D + (dt_ + 1) * 128], lambda c: hnT[:, c, 0:w], 8, [b_wci, b_hnT])
            px, b_px = next_pb(0, 4)
            mm_group(px[:, 0:w], b_px, lambda c, dt_=dt_: Wci[:, c, 2 * D + dt_ * 128:2 * D + (dt_ + 1) * 128], lambda c: hnT[:, c, 0:w], 8, [b_wci, b_hnT])
            cs, b_cs = csb_r.next()
            S.op("act", lambda e, cs=cs, pc_=pc_: e.copy(out=cs[:, 0:w], in_=pc_[:, 0:w]), reads=[b_pc], writes=[b_cs])
            if bi == 0:
                S.op("dve", lambda e, cs=cs, px=px, dt_=dt_: e.tensor_tensor(out=uT[:, dt_, 0:2], in0=px[:, w - 2:w], in1=cs[:, w - 2:w], op=ALU.mult),
                     reads=[b_px, b_cs], writes=[b_uT[dt_]])
                S.op("dve", lambda e, dt_=dt_: e.tensor_scalar(out=uT[:, dt_, 0:2], in0=uT[:, dt_, 0:2], scalar1=flag[:, 0:1], scalar2=None, op0=ALU.mult),
                     reads=[b_const, b_uT[dt_]], writes=[b_uT[dt_]])
                continue
            S.op("dve", lambda e, cs=cs, px=px, dt_=dt_: e.tensor_tensor(out=uT[:, dt_, 2:2 + w], in0=px[:, 0:w], in1=cs[:, 0:w], op=ALU.mult),
                 reads=[b_px, b_cs], writes=[b_uT[dt_]])
            pbb, b_pbb = next_pb(0, 4)
            mm_group(pbb[:, 0:w], b_pbb, lambda c, dt_=dt_: Wci[:, c, dt_ * 128:(dt_ + 1) * 128], lambda c: hnT[:, c, 0:w], 8, [b_wci, b_hnT])
            bs, b_bs = bsb_r.next()
            S.op("act", lambda e, bs=bs, pbb=pbb: e.copy(out=bs[:, 0:w], in_=pbb[:, 0:w]), reads=[b_pbb], writes=[b_bs])
            y, b_y = y_r.next()
            S.op("dve", lambda e, y=y, dt_=dt_: e.tensor_scalar(out=y[:, 0:w], in0=uT[:, dt_, 2:2 + w], scalar1=cw[:, dt_, 2:3], scalar2=None, op0=ALU.mult),
                 reads=[b_uT[dt_], b_cw], writes=[b_y])
            S.op("dve", lambda e, y=y, dt_=dt_: e.scalar_tensor_tensor(out=y[:, 0:w], in0=uT[:, dt_, 1:1 + w], scalar=cw[:, dt_, 1:2], in1=y[:, 0:w], op0=ALU.mult, op1=ALU.add),
                 reads=[b_uT[dt_], b_cw, b_y], writes=[b_y])
            S.op("dve", lambda e, y=y, dt_=dt_: e.scalar_tensor_tensor(out=y[:, 0:w], in0=uT[:, dt_, 0:w], scalar=cw[:, dt_, 0:1], in1=y[:, 0:w], op0=ALU.mult, op1=ALU.add),
                 reads=[b_uT[dt_], b_cw, b_y], writes=[b_y])
            S.op("dve", lambda e, y=y, bs=bs, dt_=dt_: e.tensor_tensor(out=zT[:, dt_, 0:w], in0=y[:, 0:w], in1=bs[:, 0:w], op=ALU.mult),
                 reads=[b_y, b_bs], writes=[b_zT])
            S.op("dve", lambda e, dt_=dt_: e.tensor_copy(out=uT[:, dt_, 0:2], in_=uT[:, dt_, w:w + 2]), reads=[b_uT[dt_]], writes=[b_uT[dt_]])
        if bi == 0:
            return
        for t in range(ntile):
            it = qoff // 128 + t
            for half in range(2):
                pb, b_pb = next_pb(4, 7)
                mm_group(pb[:, :], b_pb, lambda c, t=t: zT[:, c, t * 128:(t + 1) * 128],
                         lambda c, half=half: Wco[:, c, half * 512:(half + 1) * 512], 8, [b_zT, b_wco])
                S.op("dve", lambda e, t=t, half=half, pb=pb: e.tensor_tensor(out=h2[t][:, half * 512:(half + 1) * 512], in0=pb[:, :],
                                                                            in1=h2[t][:, half * 512:(half + 1) * 512], op=ALU.add),
                     reads=[b_pb, b_h2[t]], writes=[b_h2[t]])
            S.dma("sp", lambda e, t=t, it=it: e.dma_start(out=HS[it * 128:(it + 1) * 128, :], in_=h2[t]), reads=[b_h2[t]], writes=[bHS2], disjoint=True)

    for bi, (qoff, w) in enumerate(blocks):
        phaseD_block(bi, qoff, w)
    S.barrier()
    if upto == 'D':
        S.emit(nc, st)
        st.close()
        return nc

    ar.reset()
    hnT_r = Ring("hnT", [ar.take([128, 8, 512]) for _ in range(1)])
    gu_r = Ring("gu", [ar.take([128, 2, 8, FP]) for _ in range(3)])
    actT = ar.take([128, NFT, 512]); b_actT = Buf("actT")
    dn_r = Ring("dn", [ar.take([128, NFT, 512]) for _ in range(2)])
    tmp_r = Ring("tmp", [ar.take([128, 512]) for _ in range(2)])
    sq = ar.take([128, D]); b_sq = Buf("sq")
    acc = [ar.take([128, D], F32) for _ in range(4)]
    b_acc = [Buf("acc%d" % i) for i in range(4)]
    gE = ar.take([128, D], F32); b_gE = Buf("gE")
    gF = ar.take([128, D], F32); b_gF = Buf("gF")
    hnf_r = Ring("hnf", [ar.take([128, D], F32) for _ in range(2)])
    hnhi_r = Ring("hnhi", [ar.take([128, D]) for _ in range(2)])
    hnlo_r = Ring("hnlo", [ar.take([128, D]) for _ in range(2)])
    loT_r = Ring("loT", [ar.take([128, 8, 128]) for _ in range(2)])
    wr = ar.take([128, 8, NE], F32); b_wr = Buf("wr")
    wr_hi = ar.take([128, 8, NE])
    wr_lo = ar.take([128, 8, NE])
    lg = [ar.take([128, 4 * NE], F32) for _ in range(4)]
    b_lg = [Buf("lg%d" % i) for i in range(4)]
    sm = [ar.take([128, 8], F32) for _ in range(4)]
    o_r = Ring("o", [ar.take([128, D], F32) for _ in range(2)])
    S.dma("sp", lambda e: e.dma_start(out=wr, in_=moe_w_router.rearrange("(c p) n -> p c n", p=128)), writes=[b_wr])
    S.op("dve", lambda e: e.tensor_copy(out=wr_hi, in_=wr), reads=[b_wr], writes=[b_wr])
    S.op("dve", lambda e: e.tensor_tensor(out=wr_lo, in0=wr, in1=wr_hi, op=ALU.subtract), reads=[b_wr], writes=[b_wr])
    load_g(gE, moe_norm, b_gE)
    load_g(gF, final_norm, b_gF)
    def phaseE_block(qoff, w):
        ntile = w // 128
        hnT, b_hnT = hnT_r.next()
        for t in range(ntile):
            it = qoff // 128 + t
            S.dma("sp", lambda e, t=t, it=it: e.dma_start(out=acc[t], in_=HS[it * 128:(it + 1) * 128, :]), reads=[bHS2], writes=[b_acc[t]])
            hnf, b_hnf = hnf_r.next()
            norm_tile(acc[t], b_acc[t], gE, b_gE, None, None, sq, b_sq, out_f32=hnf, b_of=b_hnf)
            hi, b_hi = hnhi_r.next()
            lo_, b_lo = hnlo_r.next()
            S.op("pool", lambda e, hi=hi, hnf=hnf: e.tensor_copy(out=hi, in_=hnf), reads=[b_hnf], writes=[b_hi])
            S.op("dve", lambda e, hi=hi, lo_=lo_, hnf=hnf: e.tensor_tensor(out=lo_, in0=hnf, in1=hi, op=ALU.subtract), reads=[b_hnf, b_hi], writes=[b_lo])
            transpose_to(hi, b_hi, hnT[:, :, t * 128:(t + 1) * 128], b_hnT)
            loT, b_loT = loT_r.next()
            transpose_to(lo_, b_lo, loT[:, :, :], b_loT)
            pl, b_pl = PB[6], PBb[6]

            def rmm(e, t=t, loT=loT):
                k = 0
                for (src, wsel) in ((0, wr_hi), (1, wr_hi), (0, wr_lo)):
                    for c in range(8):
                        lhs = hnT[:, c, t * 128:(t + 1) * 128] if src == 0 else loT[:, c, :]
                        ins = e.matmul(pl[:, 0:NE], lhsT=lhs, rhs=wsel[:, c, :], start=(k == 0), stop=(k == 23))
                        k += 1
                return ins
            S.op("pe", rmm, reads=[b_hnT, b_loT, b_wr], writes=[b_pl])
            L = lg[t][:, 0:NE]; M1 = lg[t][:, NE:2 * NE]; L2 = lg[t][:, 2 * NE:3 * NE]; G = lg[t][:, 3 * NE:4 * NE]
            m1 = sm[t][:, 0:1]; m2 = sm[t][:, 1:2]; nm1 = sm[t][:, 2:3]; dd = sm[t][:, 3:4]
            bl = b_lg[t]
            S.op("dve", lambda e, L=L: e.tensor_copy(out=L, in_=pl[:, 0:NE]), reads=[b_pl], writes=[bl])
            S.op("dve", lambda e, L=L, m1=m1: e.reduce_max(out=m1, in_=L, axis=AX.X), reads=[bl], writes=[bl])
            S.op("dve", lambda e, L=L, m1=m1, M1=M1: e.tensor_scalar(out=M1, in0=L, scalar1=m1, scalar2=-1e30, op0=ALU.is_ge, op1=ALU.mult), reads=[bl], writes=[bl])
            S.op("dve", lambda e, L=L, M1=M1, L2=L2: e.tensor_tensor(out=L2, in0=L, in1=M1, op=ALU.add), reads=[bl], writes=[bl])
            S.op("dve", lambda e, L2=L2, m2=m2: e.reduce_max(out=m2, in_=L2, axis=AX.X), reads=[bl], writes=[bl])
            S.op("dve", lambda e, m1=m1, nm1=nm1: e.tensor_scalar(out=nm1, in0=m1, scalar1=-1.0, scalar2=None, op0=ALU.mult), reads=[bl], writes=[bl])
            S.op("act", lambda e, L=L, L2=L2, nm1=nm1: e.activation(out=L2, in_=L, func=AF.Exp, bias=nm1, scale=1.0), reads=[bl], writes=[bl])
            S.op("act", lambda e, m2=m2, dd=dd, nm1=nm1: e.activation(out=dd, in_=m2, func=AF.Exp, bias=nm1, scale=1.0), reads=[bl], writes=[bl])
            S.op("dve", lambda e, dd=dd: e.tensor_scalar(out=dd, in0=dd, scalar1=1.0, scalar2=None, op0=ALU.add), reads=[bl], writes=[bl])
            S.op("dve", lambda e, dd=dd: e.reciprocal(out=dd, in_=dd), reads=[bl], writes=[bl])
            S.op("dve", lambda e, L=L, m2=m2, M1=M1, dd=dd: e.tensor_scalar(out=M1, in0=L, scalar1=m2, scalar2=dd, op0=ALU.is_ge, op1=ALU.mult), reads=[bl], writes=[bl])
            S.op("dve", lambda e, G=G, M1=M1, L2=L2: e.tensor_tensor(out=G, in0=M1, in1=L2, op=ALU.mult), reads=[bl], writes=[bl])
        for e_ in range(nex):
            def consume(t, half, pb, b_pb, e_=e_):
                G = lg[t][:, 3 * NE:4 * NE]
                S.op("dve", lambda e: e.scalar_tensor_tensor(out=acc[t][:, half * 512:(half + 1) * 512], in0=pb[:, :], scalar=G[:, e_:e_ + 1],
                                                            in1=acc[t][:, half * 512:(half + 1) * 512], op0=ALU.mult, op1=ALU.add),
                     reads=[b_pb, b_acc[t], b_lg[t]], writes=[b_acc[t]])
            ffn(hnT, b_hnT, w, ntile, GM[e_], UM[e_], DM[e_], bW["G%d" % e_], bW["U%d" % e_], bW["D%d" % e_], gu_r, dn_r, actT, b_actT, tmp_r, consume)
        for t in range(ntile):
            it = qoff // 128 + t
            ot, b_ot = o_r.next()
            norm_tile(acc[t], b_acc[t], gF, b_gF, None, None, sq, b_sq, out_f32=ot, b_of=b_ot)
            S.dma("sp", lambda e, ot=ot, it=it: e.dma_start(out=out_d[(it - 1) * 128:it * 128, :], in_=ot), reads=[b_ot], writes=[bOUT], disjoint=True)

    for (qoff, w) in blocks[1:]:
        phaseE_block(qoff, w)
    S.wait_all("sp", [bOUT])
    S.emit(nc, st)
    st.close()
    return nc


_NC_CACHE = {}


def _consts():
    ident = np.eye(128, dtype=np.float32)
    k = np.arange(128)[:, None]
    q = np.arange(128)[None, :]
    masktri = np.where(k > q, np.float32(-30000.0), np.float32(0.0)).astype(np.float32)
    return ident, masktri


def _weights_map(inputs):
    f = lambda a: np.ascontiguousarray(np.asarray(a, dtype=np.float32))
    m = {
        "attn_norm": f(inputs["attn_norm"]).reshape(1, D),
        "attn_w_in": f(inputs["attn_w_in"]).reshape(D, 3 * D + NH),
        "attn_b_forget": f(inputs["attn_b_forget"]).reshape(NH, 1),
        "attn_w_out": f(inputs["attn_w_out"]).reshape(D, D),
        "ffn_norm": f(inputs["ffn_norm"]).reshape(1, D),
        "ffn_w_gate": f(inputs["ffn_w_gate"]).reshape(D, F),
        "ffn_w_up": f(inputs["ffn_w_up"]).reshape(D, F),
        "ffn_w_down": f(inputs["ffn_w_down"]).reshape(F, D),
        "conv_norm": f(inputs["conv_norm"]).reshape(1, D),
        "conv_w_in": f(inputs["conv_w_in"]).reshape(D, 3 * D),
        "conv_wT": np.ascontiguousarray(f(inputs["conv_w"]).reshape(3, D).T),
        "conv_w_out": f(inputs["conv_w_out"]).reshape(D, D),
        "moe_norm": f(inputs["moe_norm"]).reshape(1, D),
        "moe_w_router": f(inputs["moe_w_router"]).reshape(D, NE),
        "moe_w_gate": f(inputs["moe_w_gate"]).reshape(NE, D, F),
        "moe_w_up": f(inputs["moe_w_up"]).reshape(NE, D, F),
        "moe_w_down": f(inputs["moe_w_down"]).reshape(NE, F, D),
        "final_norm": f(inputs["final_norm"]).reshape(1, D),
    }
    ident, masktri = _consts()
    m["ident"] = ident
    m["masktri"] = masktri
    return m


def kernel(**inputs):
    x = np.asarray(inputs["x"], dtype=np.float32)
    B, SEQ, _ = x.shape
    half = SEQ // 2
    NBO = half // 512
    key = (NBO, NBO)
    if key not in _NC_CACHE:
        _NC_CACHE[key] = build(NBO, NBO)
    nc = _NC_CACHE[key]
    wm = _weights_map(inputs)
    in_maps = []
    cores = []
    for b in range(B):
        for hf in range(2):
            m = dict(wm)
            m["xo"] = np.ascontiguousarray(x[b, hf * half:(hf + 1) * half])
            if hf == 0:
                m["xp"] = np.zeros((half, D), np.float32)
                m["flag"] = np.zeros((128, 1), np.float32)
            else:
                m["xp"] = np.ascontiguousarray(x[b, 0:half])
                m["flag"] = np.ones((128, 1), np.float32)
            in_maps.append(m)
            cores.append((b, hf))
    res = run_bass_kernel_spmd(nc, in_maps, core_ids=list(range(len(in_maps))))
    out = np.empty((B, SEQ, D), np.float32)
    for (b, hf), r in zip(cores, res.results):
        out[b, hf * half:(hf + 1) * half] = r["out"]
    return out
```
